# Optimizing a Trainium2 kernel written in Bass

```python
import jax, jax.numpy as jnp
from jax import lax
import numpy as np

D_MODEL = 2048
BATCH = 1
SEQ = 16384
DEPTH = 2

HEAD_DIM = 128
ROPE_DIM = HEAD_DIM // 4
ROPE_THETA = 500000.0
A_Q_HEADS = 16
A_KV_HEADS = 4
IDX_HEADS = 16
IDX_DIM = 64
IDX_ROPE_DIM = IDX_DIM // 4
TOPK_MAX = 256
B_GROUPS = ((128, 1), (512, 4), (2048, 16))
B_Q_HEADS = 16
B_KV_HEADS = 4
D_FF = 4 * D_MODEL
Q_BLOCK = 128
EPS = 1e-6
N_A = DEPTH // 2
N_B = DEPTH - N_A

A_Q_W = A_Q_HEADS * HEAD_DIM
A_KV_W = A_KV_HEADS * HEAD_DIM
A_QI_W = IDX_HEADS * IDX_DIM
A_IN_W = A_Q_W + 2 * A_KV_W + A_QI_W + IDX_DIM + IDX_HEADS
A_SPLITS = (A_Q_W, A_Q_W + A_KV_W, A_Q_W + 2 * A_KV_W, A_Q_W + 2 * A_KV_W + A_QI_W,
            A_Q_W + 2 * A_KV_W + A_QI_W + IDX_DIM)
B_Q_W = len(B_GROUPS) * B_Q_HEADS * HEAD_DIM
B_KV_W = B_KV_HEADS * HEAD_DIM
B_OUT_W = B_Q_HEADS * HEAD_DIM

kernel_name = "yoco_dsa_dilated_hybrid"


def rmsnorm(x, g):
    x32 = x.astype(jnp.float32)
    y = x32 * lax.rsqrt(jnp.mean(x32 * x32, axis=-1, keepdims=True) + EPS)
    return (y * g.astype(jnp.float32)).astype(x.dtype)


def rope_tables(seq, rot_dim):
    inv = ROPE_THETA ** (-jnp.arange(0, rot_dim, 2, dtype=jnp.float32) / rot_dim)
    ang = jnp.arange(seq, dtype=jnp.float32)[:, None] * inv[None, :]
    return jnp.cos(ang), jnp.sin(ang)


def apply_rope(x, cos, sin):
    half = cos.shape[-1]
    c = cos[None, :, None, :].astype(x.dtype)
    s = sin[None, :, None, :].astype(x.dtype)
    x1, x2, xp = x[..., :half], x[..., half:2 * half], x[..., 2 * half:]
    return jnp.concatenate([x1 * c - x2 * s, x2 * c + x1 * s, xp], axis=-1)


def to_blocks(a):
    b, s = a.shape[:2]
    return jnp.moveaxis(a.reshape(b, s // Q_BLOCK, Q_BLOCK, *a.shape[2:]), 1, 0)


def from_blocks(a):
    a = jnp.moveaxis(a, 0, 1)
    return a.reshape(a.shape[0], a.shape[1] * a.shape[2], *a.shape[3:])


def dsa_mixer(h, w_in, w_out, rope_h, rope_i):
    b, s, _ = h.shape
    rep = A_Q_HEADS // A_KV_HEADS
    topk = min(TOPK_MAX, s // 4)
    q, k, v, qi, ki, wi = jnp.split(h @ w_in, A_SPLITS, axis=-1)
    q = apply_rope(q.reshape(b, s, A_Q_HEADS, HEAD_DIM), *rope_h).reshape(b, s, A_KV_HEADS, rep, HEAD_DIM)
    k = apply_rope(k.reshape(b, s, A_KV_HEADS, HEAD_DIM), *rope_h)
    v = v.reshape(b, s, A_KV_HEADS, HEAD_DIM)
    qi = apply_rope(qi.reshape(b, s, IDX_HEADS, IDX_DIM), *rope_i)
    ki = apply_rope(ki.reshape(b, s, 1, IDX_DIM), *rope_i)[:, :, 0]
    wi = wi * (IDX_HEADS ** -0.5)
    key_pos = jnp.arange(s)
    bidx = jnp.arange(b)[:, None, None]

    def block(args):
        qb, qib, wib, tq = args
        isc = jnp.einsum('bqhd,bsd->bqhs', qib, ki, preferred_element_type=jnp.float32) * (IDX_DIM ** -0.5)
        isc = jnp.einsum('bqhs,bqh->bqs', jax.nn.relu(isc), wib.astype(jnp.float32))
        isc = jnp.where((key_pos[None, :] <= tq[:, None])[None], isc, -jnp.inf)
        _, sel = lax.top_k(isc, topk)
        kg = k[bidx, sel]
        vg = v[bidx, sel]
        sc = jnp.einsum('bqgrd,bqkgd->bqgrk', qb, kg, preferred_element_type=jnp.float32) * (HEAD_DIM ** -0.5)
        valid = (sel <= tq[None, :, None])[:, :, None, None, :]
        p = jax.nn.softmax(jnp.where(valid, sc, -jnp.inf), axis=-1)
        o = jnp.einsum('bqgrk,bqkgd->bqgrd', p.astype(vg.dtype), vg)
        return o.reshape(b, Q_BLOCK, A_Q_W)

    tpos = key_pos.reshape(s // Q_BLOCK, Q_BLOCK)
    out = lax.map(block, (to_blocks(q), to_blocks(qi), to_blocks(wi), tpos))
    return from_blocks(out) @ w_out


def dilated_mixer(h, w_q, w_out, k_sh, v_sh, rope_h):
    b, s, _ = h.shape
    ng = len(B_GROUPS)
    rep = B_Q_HEADS // B_KV_HEADS
    q = apply_rope((h @ w_q).reshape(b, s, ng * B_Q_HEADS, HEAD_DIM), *rope_h)
    q = q.reshape(b, s, ng, B_KV_HEADS, rep, HEAD_DIM)

    def block(args):
        qb, tq = args
        outs, lses = [], []
        for g, (win, dil) in enumerate(B_GROUPS):
            pos = tq[:, None] - dil * jnp.arange(win // dil + 1)[None, :]
            valid = (pos >= 0)[None, :, None, None, :]
            pos = jnp.maximum(pos, 0)
            kg = k_sh[:, pos]
            vg = v_sh[:, pos]
            sc = jnp.einsum('bqhrd,bqnhd->bqhrn', qb[:, :, g], kg, preferred_element_type=jnp.float32) * (HEAD_DIM ** -0.5)
            sc = jnp.where(valid, sc, -jnp.inf)
            m = jnp.max(sc, axis=-1, keepdims=True)
            e = jnp.exp(sc - m)
            den = jnp.sum(e, axis=-1, keepdims=True)
            o = jnp.einsum('bqhrn,bqnhd->bqhrd', (e / den).astype(vg.dtype), vg)
            outs.append(o.astype(jnp.float32))
            lses.append((m + jnp.log(den))[..., 0])
        wts = jax.nn.softmax(jnp.stack(lses, axis=0), axis=0)
        o = jnp.einsum('gbqhr,gbqhrd->bqhrd', wts, jnp.stack(outs, axis=0))
        return o.reshape(b, Q_BLOCK, B_OUT_W).astype(h.dtype)

    tpos = jnp.arange(s).reshape(s // Q_BLOCK, Q_BLOCK)
    out = lax.map(block, (to_blocks(q), tpos))
    return from_blocks(out) @ w_out


def sq_relu_mlp(h, w_up, w_down):
    u = jax.nn.relu(h @ w_up)
    return (u * u) @ w_down


def setup_inputs(seed: int = 0) -> dict:
    key = jax.random.key(seed)
    ks = jax.random.split(key, 13)

    def w(k, shape):
        return jax.random.normal(k, shape, jnp.float32) * (shape[-2] ** -0.5)

    def gain(k, shape):
        return 1.0 + 0.01 * jax.random.normal(k, shape, jnp.float32)

    return {
        'x': jax.random.normal(ks[0], (BATCH, SEQ, D_MODEL), jnp.float32),
        'a_attn_norm': gain(ks[1], (N_A, D_MODEL)),
        'a_w_in': w(ks[2], (N_A, D_MODEL, A_IN_W)),
        'a_w_out': w(ks[3], (N_A, A_Q_W, D_MODEL)),
        'kv_norm': gain(ks[4], (D_MODEL,)),
        'w_kv': w(ks[5], (D_MODEL, 2 * B_KV_W)),
        'b_attn_norm': gain(ks[6], (N_B, D_MODEL)),
        'b_w_q': w(ks[7], (N_B, D_MODEL, B_Q_W)),
        'b_w_out': w(ks[8], (N_B, B_OUT_W, D_MODEL)),
        'mlp_norm': gain(ks[9], (DEPTH, D_MODEL)),
        'w_up': w(ks[10], (DEPTH, D_MODEL, D_FF)),
        'w_down': w(ks[11], (DEPTH, D_FF, D_MODEL)),
        'final_norm': gain(ks[12], (D_MODEL,)),
    }


def reference(x, a_attn_norm, a_w_in, a_w_out, kv_norm, w_kv, b_attn_norm, b_w_q, b_w_out,
              mlp_norm, w_up, w_down, final_norm):
    b, s, _ = x.shape
    rope_h = rope_tables(s, ROPE_DIM)
    rope_i = rope_tables(s, IDX_ROPE_DIM)
    h = x
    k_sh = None
    v_sh = None
    for i in range(DEPTH):
        if i < N_A:
            h = h + dsa_mixer(rmsnorm(h, a_attn_norm[i]), a_w_in[i], a_w_out[i], rope_h, rope_i)
        else:
            j = i - N_A
            if j == 0:
                kv = rmsnorm(h, kv_norm) @ w_kv
                k_sh = apply_rope(kv[..., :B_KV_W].reshape(b, s, B_KV_HEADS, HEAD_DIM), *rope_h)
                v_sh = kv[..., B_KV_W:].reshape(b, s, B_KV_HEADS, HEAD_DIM)
            h = h + dilated_mixer(rmsnorm(h, b_attn_norm[j]), b_w_q[j], b_w_out[j], k_sh, v_sh, rope_h)
        h = h + sq_relu_mlp(rmsnorm(h, mlp_norm[i]), w_up[i], w_down[i])
    return rmsnorm(h, final_norm)
```

```python
from contextlib import ExitStack

import numpy as np
import ml_dtypes
import concourse.bass as bass
import concourse.mybir as mybir
from concourse.bass_utils import run_bass_kernel_spmd

F32 = mybir.dt.float32
BF16 = mybir.dt.bfloat16
AF = mybir.ActivationFunctionType
ALU = mybir.AluOpType
AX = mybir.AxisListType
NPBF = ml_dtypes.bfloat16

NCORES = 8
D = 2048
S = 16384
NTL = S // NCORES
NB = NTL // 128
HD = 128
EPS = 1e-6
A_IN_W = 4176
D_FF = 8192
TOPK = 256
NEG = -30000.0
BISECT_ITERS = 22

SAME_ENG_SYNC = True


class T:
    __slots__ = ("name", "w", "r")

    def __init__(self, name=""):
        self.name = name
        self.w = []
        self.r = []


class Chan:
    __slots__ = ("sem", "val")

    def __init__(self, sem):
        self.sem = sem
        self.val = 0


class Op:
    __slots__ = ("eng", "fn", "deps", "sig", "idx", "chan", "cval", "is_dma", "seq")


class Prog:
    ENGS = ("pe", "act", "dve", "pool", "sp")

    def __init__(self, nc, stack):
        self.nc = nc
        self.stack = stack
        self.ops = {e: [] for e in self.ENGS}
        self.esem = {e: stack.enter_context(nc.semaphore("S_" + e)) for e in self.ENGS}
        self.nchan = 0
        self.nname = 0
        self.nseq = 0

    def sb(self, shape, dt, name=None):
        self.nname += 1
        return self.stack.enter_context(self.nc.sbuf_tensor(name or ("sb%d" % self.nname), shape, dt))

    def ps(self, shape, dt, name=None):
        self.nname += 1
        return self.stack.enter_context(self.nc.psum_tensor(name or ("ps%d" % self.nname), shape, dt))

    def chan(self):
        self.nchan += 1
        return Chan(self.stack.enter_context(self.nc.semaphore("C%d" % self.nchan)))

    def _mk(self, eng, fn, reads, writes, pwrites):
        o = Op()
        o.eng = eng
        o.fn = fn
        o.sig = False
        o.idx = None
        o.is_dma = False
        o.chan = None
        o.cval = None
        deps = []
        for t in reads:
            deps.extend(t.w)
        for t in writes:
            deps.extend(t.w)
            deps.extend(t.r)
        for t in pwrites:
            deps.extend(t.w[:1])
            deps.extend(t.r)
        best = {}
        for d in deps:
            key = ("c", id(d.chan)) if d.is_dma else ("e", d.eng)
            cur = best.get(key)
            if cur is None or d.seq > cur.seq:
                best[key] = d
        o.deps = list(best.values())
        o.seq = self.nseq
        self.nseq += 1
        for t in reads:
            t.r.append(o)
        for t in writes:
            t.w = [o]
            t.r = []
        for t in pwrites:
            t.w.append(o)
        self.ops[eng].append(o)
        return o

    def op(self, eng, fn, reads=(), writes=(), pwrites=()):
        return self._mk(eng, fn, reads, writes, pwrites)

    def dma(self, eng, fn, chan, reads=(), writes=(), pwrites=()):
        o = self._mk(eng, fn, reads, writes, pwrites)
        o.is_dma = True
        o.chan = chan
        chan.val += 16
        o.cval = chan.val
        return o

    def emit(self, final_waits=()):
        for e in self.ENGS:
            for o in self.ops[e]:
                for d in o.deps:
                    if d.is_dma:
                        continue
                    if d.eng == o.eng and (not SAME_ENG_SYNC or d.eng == "pe"):
                        continue
                    d.sig = True
        for e in self.ENGS:
            k = 0
            for o in self.ops[e]:
                if not o.is_dma and o.sig:
                    k += 1
                    o.idx = k
        with self.nc.Block() as block:
            for e in self.ENGS:
                self._emit_engine(block, e, final_waits)

    def _emit_engine(self, block, e, final_waits):
        ops = self.ops[e]
        esem = self.esem
        deco = {"pe": block.tensor, "act": block.scalar, "dve": block.vector,
                "pool": block.gpsimd, "sp": block.sync}[e]

        @deco
        def _(eng):
            waited = {}
            for o in ops:
                need = {}
                for d in o.deps:
                    if d.is_dma:
                        key = ("c", id(d.chan))
                        sem, val = d.chan.sem, d.cval
                    else:
                        if d.eng == e and (not SAME_ENG_SYNC or e == "pe"):
                            continue
                        key = ("e", d.eng)
                        sem, val = esem[d.eng], d.idx
                    if waited.get(key, 0) >= val:
                        continue
                    if key not in need or need[key][1] < val:
                        need[key] = (sem, val)
                for key, (sem, val) in need.items():
                    eng.wait_ge(sem, val)
                    waited[key] = val
                inst = o.fn(eng)
                if o.is_dma:
                    inst.then_inc(o.chan.sem, 16)
                elif o.sig:
                    inst.then_inc(esem[e], 1)
            if e == "sp":
                for ch in final_waits:
                    eng.wait_ge(ch.sem, ch.val)


class Buf:
    __slots__ = ("t", "d", "c")

    def __init__(self, t, c=None):
        self.t = t
        self.d = T()
        self.c = c


class Ring:
    def __init__(self, bufs):
        self.bufs = bufs
        self.i = 0

    def next(self):
        b = self.bufs[self.i % len(self.bufs)]
        self.i += 1
        return b


def W(first):
    return "writes" if first else "pwrites"


class Ctx:
    def __init__(self, nc, st):
        self.nc = nc
        self.P = Prog(nc, st)
        self.out_chans = []

    def load_const(self, dram_ap, shape, dt, eng="sp"):
        P = self.P
        b = Buf(P.sb(shape, dt), P.chan())
        P.dma(eng, lambda e: e.dma_start(out=b.t[:], in_=dram_ap), b.c, writes=[b.d])
        return b


def norm_transpose(cx, src_rows, nblk, gbc, hT, hTd, res, ident, scale_only=None):
    P = cx.P
    for b in range(nblk):
        xt = res["xt"].next()
        P.dma("sp", (lambda xt=xt, b=b: lambda e: e.dma_start(out=xt.t[:], in_=src_rows(b)))(), xt.c, writes=[xt.d])
        junk = res["junk"]
        ss = res["ss"].next()
        P.op("act", (lambda xt=xt, ss=ss: lambda e: e.activation(out=junk.t[:], in_=xt.t[:], func=AF.Square, accum_out=ss.t[:]))(),
             reads=[xt.d], writes=[junk.d, ss.d])
        P.op("act", (lambda ss=ss: lambda e: e.activation(out=ss.t[:], in_=ss.t[:], func=AF.Sqrt, scale=1.0 / D, bias=res["eps"].t[:]))(),
             reads=[ss.d, res["eps"].d], writes=[ss.d])
        P.op("dve", (lambda ss=ss: lambda e: e.reciprocal(out=ss.t[:], in_=ss.t[:]))(), reads=[ss.d], writes=[ss.d])
        xn = res["xn"].next()
        P.op("dve", (lambda xt=xt, ss=ss, xn=xn: lambda e: e.scalar_tensor_tensor(
            out=xn.t[:], in0=xt.t[:], scalar=ss.t[:], in1=gbc.t[:], op0=ALU.mult, op1=ALU.mult))(),
            reads=[xt.d, ss.d, gbc.d], writes=[xn.d])
        transpose_block(cx, xn, hT, hTd, b, res, ident, first=(b == 0))


def transpose_block(cx, xn, hT, hTd, b, res, ident, first):
    P = cx.P
    for q in range(4):
        pt = res["pT"].next()
        for i in range(4):
            kc = q * 4 + i
            P.op("pe", (lambda pt=pt, i=i, kc=kc: lambda e: e.transpose(
                out=pt.t[:, i, :], in_=xn.t[:, kc * 128:(kc + 1) * 128], identity=ident.t[:]))(),
                reads=[xn.d, ident.d], **{W(i == 0): [pt.d]})
        eng = "act" if q % 2 == 0 else "dve"
        fn = (lambda pt=pt, q=q: (lambda e: e.copy(out=hT[:, q * 4:(q + 1) * 4, b * 128:(b + 1) * 128], in_=pt.t[:])))() if eng == "act" else \
             (lambda pt=pt, q=q: (lambda e: e.tensor_copy(out=hT[:, q * 4:(q + 1) * 4, b * 128:(b + 1) * 128], in_=pt.t[:])))()
        P.op(eng, fn, reads=[pt.d], **{W(first and q == 0): [hTd]})


def norm_res(cx):
    P = cx.P
    res = {}
    res["xt"] = Ring([Buf(P.sb([128, D], F32), P.chan()) for _ in range(2)])
    res["junk"] = Buf(P.sb([128, D], BF16))
    res["ss"] = Ring([Buf(P.sb([128, 1], F32)) for _ in range(2)])
    res["xn"] = Ring([Buf(P.sb([128, D], BF16)) for _ in range(2)])
    res["pT"] = Ring([Buf(P.ps([128, 4, 128], BF16)) for _ in range(2)])
    eps = Buf(P.sb([128, 1], F32))
    P.op("pool", lambda e: e.memset(eps.t[:], EPS), writes=[eps.d])
    res["eps"] = eps
    return res


def stream_linear(cx, hT, hTd, ntok_blocks, w_dram, col0, ncols, wring, pring, consume, chunk=512):
    P = cx.P
    wv = w_dram.rearrange("(kc k) n -> k kc n", k=128)
    nch = (ncols + chunk - 1) // chunk
    for ci in range(nch):
        c0 = col0 + ci * chunk
        cw = min(chunk, col0 + ncols - c0)
        wb = wring.next()
        for half in range(2):
            P.dma("pool", (lambda wb=wb, c0=c0, cw=cw, half=half: lambda e: e.dma_start(
                out=wb.t[:, half * 8:(half + 1) * 8, 0:cw], in_=wv[:, half * 8:(half + 1) * 8, c0:c0 + cw]))(),
                wb.c, **{W(half == 0): [wb.d]})
        for tb in range(ntok_blocks):
            pb = pring.next()
            for kc in range(16):
                P.op("pe", (lambda pb=pb, wb=wb, kc=kc, tb=tb, cw=cw: lambda e: e.matmul(
                    pb.t[:, 0:cw], lhsT=hT[:, kc, tb * 128:(tb + 1) * 128], rhs=wb.t[:, kc, 0:cw],
                    start=(kc == 0), stop=(kc == 15)))(),
                    reads=[hTd, wb.d], **{W(kc == 0): [pb.d]})
            consume(ci, c0, cw, tb, pb)


def rope_evac(cx, pb, dst, nh, hd, half, cosb, sinb, tb, tmps, col_off=0):
    P = cx.P
    n = nh * hd
    stg = tmps[4]
    P.op("act", lambda e: e.copy(out=stg.t[:, 0:n], in_=pb.t[:, col_off:col_off + n]), reads=[pb.d], writes=[stg.d])
    src = stg.t[:, 0:n].rearrange("p (h d) -> p h d", h=nh)
    dv = dst.t[:, col_off:col_off + n].rearrange("p (h d) -> p h d", h=nh)
    x1 = src[:, :, 0:half]
    x2 = src[:, :, half:2 * half]
    C = cosb.t[:, tb, :].unsqueeze(1).to_broadcast([128, nh, half])
    Sn = sinb.t[:, tb, :].unsqueeze(1).to_broadcast([128, nh, half])
    t1, t2, t3, t4 = tmps[:4]
    v = lambda t: t.t[:, 0:nh * half].rearrange("p (h d) -> p h d", h=nh)
    P.op("dve", lambda e: e.tensor_copy(out=dst.t[:, col_off:col_off + n], in_=stg.t[:, 0:n]), reads=[stg.d], writes=[dst.d])
    P.op("dve", lambda e: e.tensor_tensor(out=v(t1), in0=x1, in1=C, op=ALU.mult), reads=[stg.d, cosb.d], writes=[t1.d])
    P.op("dve", lambda e: e.tensor_tensor(out=v(t2), in0=x2, in1=Sn, op=ALU.mult), reads=[stg.d, sinb.d], writes=[t2.d])
    P.op("dve", lambda e: e.tensor_tensor(out=v(t3), in0=x2, in1=C, op=ALU.mult), reads=[stg.d, cosb.d], writes=[t3.d])
    P.op("dve", lambda e: e.tensor_tensor(out=v(t4), in0=x1, in1=Sn, op=ALU.mult), reads=[stg.d, sinb.d], writes=[t4.d])
    P.op("dve", lambda e: e.tensor_tensor(out=dv[:, :, 0:half], in0=v(t1), in1=v(t2), op=ALU.subtract),
         reads=[t1.d, t2.d], pwrites=[dst.d])
    P.op("dve", lambda e: e.tensor_tensor(out=dv[:, :, half:2 * half], in0=v(t3), in1=v(t4), op=ALU.add),
         reads=[t3.d, t4.d], pwrites=[dst.d])


def build_proj(w_cols, segs, name):
    nc = bass.Bass("TRN2", target_bir_lowering=False)
    x = nc.dram_tensor("x", [NTL, D], F32, kind="ExternalInput").ap()
    w = nc.dram_tensor("w", [D, w_cols], F32, kind="ExternalInput").ap()
    g = nc.dram_tensor("g", [1, D], F32, kind="ExternalInput").ap()
    identd = nc.dram_tensor("ident", [128, 128], BF16, kind="ExternalInput").ap()
    cosh = nc.dram_tensor("cos_h", [NTL, 16], F32, kind="ExternalInput").ap()
    sinh = nc.dram_tensor("sin_h", [NTL, 16], F32, kind="ExternalInput").ap()
    cosi = nc.dram_tensor("cos_i", [NTL, 8], F32, kind="ExternalInput").ap()
    sini = nc.dram_tensor("sin_i", [NTL, 8], F32, kind="ExternalInput").ap()
    y = nc.dram_tensor("proj", [NTL, w_cols], BF16, kind="ExternalOutput").ap()
    with ExitStack() as st:
        cx = Ctx(nc, st)
        P = cx.P
        ident = cx.load_const(identd, [128, 128], BF16)
        gbc = cx.load_const(g.broadcast_to([128, D]), [128, D], F32)
        tabs = {}
        for nm, ap, hf in (("cos_h", cosh, 16), ("sin_h", sinh, 16), ("cos_i", cosi, 8), ("sin_i", sini, 8)):
            tabs[nm] = cx.load_const(ap.rearrange("(b p) f -> p b f", p=128), [128, NB, hf], F32)
        res = norm_res(cx)
        hT = P.sb([128, 16, NTL], BF16)
        hTd = T()
        norm_transpose(cx, lambda b: x[b * 128:(b + 1) * 128, :], NB, gbc, hT, hTd, res, ident)
        wring = Ring([Buf(P.sb([128, 16, 512], BF16), P.chan()) for _ in range(2)])
        pring = Ring([Buf(P.ps([128, 512], F32)) for _ in range(2)])
        oring = Ring([Buf(P.sb([128, 512], BF16), P.chan()) for _ in range(3)])
        tmps = [Buf(P.sb([128, 128], F32)) for _ in range(4)] + [Buf(P.sb([128, 512], F32))]
        for (col0, ncols, kind) in segs:
            def consume(ci, c0, cw, tb, pb, kind=kind):
                ob = oring.next()
                if kind == "plain":
                    P.op("act", lambda e: e.copy(out=ob.t[:, 0:cw], in_=pb.t[:, 0:cw]), reads=[pb.d], writes=[ob.d])
                elif kind == "rope_h":
                    rope_evac(cx, pb, ob, cw // 128, 128, 16, tabs["cos_h"], tabs["sin_h"], tb, tmps)
                elif kind == "rope_i":
                    rope_evac(cx, pb, ob, cw // 64, 64, 8, tabs["cos_i"], tabs["sin_i"], tb, tmps)
                elif kind == "ki_wi":
                    rope_evac(cx, pb, ob, 1, 64, 8, tabs["cos_i"], tabs["sin_i"], tb, tmps)
                    P.op("act", lambda e: e.activation(out=ob.t[:, 64:80], in_=pb.t[:, 64:80], func=AF.Copy, scale=1.0 / 32.0), reads=[pb.d], pwrites=[ob.d])
                P.dma("sp", lambda e: e.dma_start(out=y[tb * 128:(tb + 1) * 128, c0:c0 + cw], in_=ob.t[:, 0:cw]), ob.c, reads=[ob.d])
            stream_linear(cx, hT, hTd, NB, w, col0, ncols, wring, pring, consume)
        chans = [b.c for b in oring.bufs]
        P.emit(final_waits=chans)
    return nc


def build_mlp(final_norm):
    nc = bass.Bass("TRN2", target_bir_lowering=False)
    oT = nc.dram_tensor("oT", [128, 16, NTL], BF16, kind="ExternalInput").ap()
    x = nc.dram_tensor("x", [NTL, D], F32, kind="ExternalInput").ap()
    w_out = nc.dram_tensor("w_out", [D, D], F32, kind="ExternalInput").ap()
    g_mlp = nc.dram_tensor("g_mlp", [1, D], F32, kind="ExternalInput").ap()
    w_up = nc.dram_tensor("w_up", [D, D_FF], F32, kind="ExternalInput").ap()
    w_down = nc.dram_tensor("w_down", [D_FF, D], F32, kind="ExternalInput").ap()
    g_fin = nc.dram_tensor("g_fin", [1, D], F32, kind="ExternalInput").ap()
    identd = nc.dram_tensor("ident", [128, 128], BF16, kind="ExternalInput").ap()
    h_mid = nc.dram_tensor("h_mid", [NTL, D], F32, kind="ExternalOutput").ap()
    h_out = nc.dram_tensor("h_out", [NTL, D], F32, kind="ExternalOutput").ap()
    y = nc.dram_tensor("y", [NTL, D], F32, kind="ExternalOutput").ap() if final_norm else None
    with ExitStack() as st:
        cx = Ctx(nc, st)
        P = cx.P
        ident = cx.load_const(identd, [128, 128], BF16)
        gbc = cx.load_const(g_mlp.broadcast_to([128, D]), [128, D], F32)
        res = norm_res(cx)
        big = Buf(P.sb([128, 16 * NTL], BF16), P.chan())
        hT = big.t[:].rearrange("p (k t) -> p k t", k=16)
        uT = big.t[:].rearrange("p (f t) -> p f t", f=64)
        for half in range(2):
            P.dma("sp", (lambda half=half: lambda e: e.dma_start(out=hT[:, half * 8:(half + 1) * 8, :], in_=oT[:, half * 8:(half + 1) * 8, :]))(),
                  big.c, **{W(half == 0): [big.d]})
        wring = Ring([Buf(P.sb([128, 16, 512], BF16), P.chan()) for _ in range(2)])
        wdring = Ring([Buf(P.sb([128, 8, 512], BF16), P.chan()) for _ in range(3)])
        pring = Ring([Buf(P.ps([128, 512], F32)) for _ in range(2)])
        pacc = [Buf(P.ps([128, 512], F32)) for _ in range(4)]
        xring = Ring([Buf(P.sb([128, 512], F32), P.chan()) for _ in range(2)])
        oring = Ring([Buf(P.sb([128, 512], F32), P.chan()) for _ in range(3)])
        rring = Ring([Buf(P.sb([128, 512], F32)) for _ in range(2)])
        hmid_d = [T() for _ in range(NB)]
        hout_d = [T() for _ in range(NB)]

        def consume_a(ci, c0, cw, tb, pb):
            xb = xring.next()
            P.dma("sp", lambda e: e.dma_start(out=xb.t[:], in_=x[tb * 128:(tb + 1) * 128, c0:c0 + cw]), xb.c, writes=[xb.d])
            ob = oring.next()
            P.op("dve", lambda e: e.tensor_tensor(out=ob.t[:], in0=pb.t[:], in1=xb.t[:], op=ALU.add), reads=[pb.d, xb.d], writes=[ob.d])
            P.dma("sp", lambda e: e.dma_start(out=h_mid[tb * 128:(tb + 1) * 128, c0:c0 + cw], in_=ob.t[:]), ob.c,
                  reads=[ob.d], pwrites=[hmid_d[tb]])
        stream_linear(cx, hT, big.d, NB, w_out, 0, D, wring, pring, consume_a)

        hnT = Buf(P.sb([128, 16, 512], BF16))
        wu = w_up.rearrange("(kc k) n -> k kc n", k=128)
        wd = w_down.rearrange("(fc p) d -> p fc d", p=128)
        for tg in range(NB // 4):
            def src_rows(b, tg=tg):
                return h_mid[(tg * 4 + b) * 128:(tg * 4 + b + 1) * 128, :]
            for b in range(4):
                hmid_d[tg * 4 + b].r = hmid_d[tg * 4 + b].r
            norm_transpose_dep(cx, src_rows, 4, gbc, hnT.t, hnT.d, res, ident, [hmid_d[tg * 4 + b] for b in range(4)])
            for fcg in range(16):
                wb = wring.next()
                for half in range(2):
                    P.dma("pool", (lambda wb=wb, fcg=fcg, half=half: lambda e: e.dma_start(
                        out=wb.t[:, half * 8:(half + 1) * 8, :], in_=wu[:, half * 8:(half + 1) * 8, fcg * 512:(fcg + 1) * 512]))(),
                        wb.c, **{W(half == 0): [wb.d]})
                for fc in range(4):
                    pu = pring.next()
                    for kc in range(16):
                        P.op("pe", (lambda pu=pu, wb=wb, kc=kc, fc=fc: lambda e: e.matmul(
                            pu.t[:], lhsT=wb.t[:, kc, fc * 128:(fc + 1) * 128], rhs=hnT.t[:, kc, :],
                            start=(kc == 0), stop=(kc == 15)))(), reads=[wb.d, hnT.d], **{W(kc == 0): [pu.d]})
                    rb = rring.next()
                    P.op("act", (lambda pu=pu, rb=rb: lambda e: e.activation(out=rb.t[:], in_=pu.t[:], func=AF.Relu))(),
                         reads=[pu.d], writes=[rb.d])
                    f = fcg * 4 + fc
                    P.op("dve", (lambda rb=rb, f=f: lambda e: e.tensor_tensor(out=uT[:, f, :], in0=rb.t[:], in1=rb.t[:], op=ALU.mult))(),
                         reads=[rb.d], **{W(f == 0): [big.d]})
            for dc in range(4):
                for piece in range(8):
                    wdb = wdring.next()
                    P.dma("pool", (lambda wdb=wdb, piece=piece, dc=dc: lambda e: e.dma_start(
                        out=wdb.t[:], in_=wd[:, piece * 8:(piece + 1) * 8, dc * 512:(dc + 1) * 512]))(), wdb.c, writes=[wdb.d])
                    for fci in range(8):
                        fc = piece * 8 + fci
                        for tb in range(4):
                            P.op("pe", (lambda wdb=wdb, fci=fci, fc=fc, tb=tb: lambda e: e.matmul(
                                pacc[tb].t[:], lhsT=uT[:, fc, tb * 128:(tb + 1) * 128], rhs=wdb.t[:, fci, :],
                                start=(fc == 0), stop=(fc == 63)))(), reads=[big.d, wdb.d], **{W(fc == 0): [pacc[tb].d]})
                for tb in range(4):
                    gb = tg * 4 + tb
                    xb = xring.next()
                    P.dma("sp", (lambda xb=xb, gb=gb, dc=dc: lambda e: e.dma_start(
                        out=xb.t[:], in_=h_mid[gb * 128:(gb + 1) * 128, dc * 512:(dc + 1) * 512]))(), xb.c,
                        reads=[hmid_d[gb]], writes=[xb.d])
                    ob = oring.next()
                    P.op("dve", (lambda ob=ob, xb=xb, tb=tb: lambda e: e.tensor_tensor(
                        out=ob.t[:], in0=pacc[tb].t[:], in1=xb.t[:], op=ALU.add))(), reads=[pacc[tb].d, xb.d], writes=[ob.d])
                    P.dma("sp", (lambda ob=ob, gb=gb, dc=dc: lambda e: e.dma_start(
                        out=h_out[gb * 128:(gb + 1) * 128, dc * 512:(dc + 1) * 512], in_=ob.t[:]))(), ob.c,
                        reads=[ob.d], pwrites=[hout_d[gb]])
        chans = [b.c for b in oring.bufs]
        if final_norm:
            gfc = gbc
            P.dma("sp", lambda e: e.dma_start(out=gbc.t[:], in_=g_fin.broadcast_to([128, D])), gbc.c, writes=[gbc.d])
            ychans = [P.chan(), P.chan()]
            for b in range(NB):
                xt = res["xt"].next()
                P.dma("sp", (lambda xt=xt, b=b: lambda e: e.dma_start(out=xt.t[:], in_=h_out[b * 128:(b + 1) * 128, :]))(), xt.c,
                      reads=[hout_d[b]], writes=[xt.d])
                ss = res["ss"].next()
                junk = res["junk"]
                P.op("act", (lambda xt=xt, ss=ss: lambda e: e.activation(out=junk.t[:], in_=xt.t[:], func=AF.Square, accum_out=ss.t[:]))(),
                     reads=[xt.d], writes=[junk.d, ss.d])
                P.op("act", (lambda ss=ss: lambda e: e.activation(out=ss.t[:], in_=ss.t[:], func=AF.Sqrt, scale=1.0 / D, bias=res["eps"].t[:]))(),
                     reads=[ss.d, res["eps"].d], writes=[ss.d])
                P.op("dve", (lambda ss=ss: lambda e: e.reciprocal(out=ss.t[:], in_=ss.t[:]))(), reads=[ss.d], writes=[ss.d])
                P.op("dve", (lambda xt=xt, ss=ss: lambda e: e.scalar_tensor_tensor(
                    out=xt.t[:], in0=xt.t[:], scalar=ss.t[:], in1=gfc.t[:], op0=ALU.mult, op1=ALU.mult))(),
                    reads=[ss.d, gfc.d], writes=[xt.d])
                P.dma("sp", (lambda xt=xt, b=b: lambda e: e.dma_start(out=y[b * 128:(b + 1) * 128, :], in_=xt.t[:]))(), ychans[b % 2], reads=[xt.d])
            chans += ychans
        P.emit(final_waits=chans)
    return nc


def norm_transpose_dep(cx, src_rows, nblk, gbc, hT, hTd, res, ident, src_deps):
    P = cx.P
    for b in range(nblk):
        xt = res["xt"].next()
        P.dma("sp", (lambda xt=xt, b=b: lambda e: e.dma_start(out=xt.t[:], in_=src_rows(b)))(), xt.c,
              reads=[src_deps[b]], writes=[xt.d])
        junk = res["junk"]
        ss = res["ss"].next()
        P.op("act", (lambda xt=xt, ss=ss: lambda e: e.activation(out=junk.t[:], in_=xt.t[:], func=AF.Square, accum_out=ss.t[:]))(),
             reads=[xt.d], writes=[junk.d, ss.d])
        P.op("act", (lambda ss=ss: lambda e: e.activation(out=ss.t[:], in_=ss.t[:], func=AF.Sqrt, scale=1.0 / D, bias=res["eps"].t[:]))(),
             reads=[ss.d, res["eps"].d], writes=[ss.d])
        P.op("dve", (lambda ss=ss: lambda e: e.reciprocal(out=ss.t[:], in_=ss.t[:]))(), reads=[ss.d], writes=[ss.d])
        xn = res["xn"].next()
        P.op("dve", (lambda xt=xt, ss=ss, xn=xn: lambda e: e.scalar_tensor_tensor(
            out=xn.t[:], in0=xt.t[:], scalar=ss.t[:], in1=gbc.t[:], op0=ALU.mult, op1=ALU.mult))(),
            reads=[xt.d, ss.d, gbc.d], writes=[xn.d])
        transpose_block(cx, xn, hT, hTd, b, res, ident, first=(b == 0))


def launch_mlp(oT_list, x_list, w_out, g_mlp, w_up, w_down, g_fin, final_norm):
    nc = build_mlp(final_norm)
    in_maps = [{"oT": oT_list[c], "x": x_list[c], "w_out": w_out, "g_mlp": g_mlp.reshape(1, D), "w_up": w_up,
                "w_down": w_down, "g_fin": g_fin.reshape(1, D), "ident": _IDENT} for c in range(NCORES)]
    res = run(nc, in_maps)
    return [r["h_out"] for r in res], ([r["y"] for r in res] if final_norm else None), [r["h_mid"] for r in res]


SCALE = float(HD) ** -0.5
DIL_GROUPS = ((128, 1, (15, 16)), (512, 4, (12, 13, 14, 15, 16)), (2048, 16, tuple(range(17))))
NMASK = sum(len(g[2]) for g in DIL_GROUPS)


def build_attn(mode):
    dsa = mode == "dsa"
    NH = 16 if dsa else 48
    nc = bass.Bass("TRN2", target_bir_lowering=False)
    qT = nc.dram_tensor("qT", [NB, 128, NH, 128], BF16, kind="ExternalInput").ap()
    identd = nc.dram_tensor("ident", [128, 128], BF16, kind="ExternalInput").ap()
    if dsa:
        kT = nc.dram_tensor("kT", [4, 128, 128, 128], BF16, kind="ExternalInput").ap()
        va = nc.dram_tensor("va", [128, 128, 4, 132], BF16, kind="ExternalInput").ap()
        qiT = nc.dram_tensor("qiT", [NB, 128, 8, 128], BF16, kind="ExternalInput").ap()
        kiT2 = nc.dram_tensor("kiT2", [128, S], BF16, kind="ExternalInput").ap()
        wid = nc.dram_tensor("wi", [NTL, 16], BF16, kind="ExternalInput").ap()
        cbd = nc.dram_tensor("cbias", [128, 1024], F32, kind="ExternalInput").ap()
    else:
        kT = nc.dram_tensor("kT", [4, 128, NB, 17, 128], BF16, kind="ExternalInput").ap()
        va = nc.dram_tensor("va", [128, NB, 17, 4, 132], BF16, kind="ExternalInput").ap()
        mbd = nc.dram_tensor("mbias", [128, NMASK, 128], BF16, kind="ExternalInput").ap()
    od = nc.dram_tensor("o", [NTL, D], BF16, kind="ExternalOutput").ap()
    with ExitStack() as st:
        cx = Ctx(nc, st)
        P = cx.P
        ident = cx.load_const(identd, [128, 128], BF16)
        psS = Ring([Buf(P.ps([128, 512], F32)) for _ in range(2)])
        acc = [Buf(P.ps([128, 512], F32)) for _ in range(4)]
        pTr = Ring([Buf(P.sb([128, 512], BF16)) for _ in range(3)])
        stg = Ring([Buf(P.sb([128, 132], F32)) for _ in range(2)])
        rcp = Ring([Buf(P.sb([128, 1], F32)) for _ in range(2)])
        otok = Ring([Buf(P.sb([128, D], BF16), P.chan()) for _ in range(2)])
        qring = Ring([Buf(P.sb([128, NH, 128], BF16), P.chan()) for _ in range(2)])
        SUB = 16 if dsa else 17
        kring = Ring([Buf(P.sb([128, SUB, 128], BF16), P.chan()) for _ in range(2)])
        vring = Ring([Buf(P.sb([128, SUB, 132], BF16), P.chan()) for _ in range(2)])
        if dsa:
            psI = Buf(P.ps([128, 512], F32))
            psT = Buf(P.ps([128, 8, 128], BF16))
            ki = cx.load_const(kiT2, [128, S], BF16)
            wib = cx.load_const(wid.rearrange("(b p) h -> p b h", p=128), [128, NB, 16], BF16)
            wif = Buf(P.sb([128, NB, 16], F32))
            P.op("dve", lambda e: e.tensor_copy(out=wif.t[:], in_=wib.t[:]), reads=[wib.d], writes=[wif.d])
            cb = cx.load_const(cbd, [128, 1024], F32)
            isc = Buf(P.sb([128, S], F32))
            mbT = Buf(P.sb([128, 128, 128], BF16))
            junk = Buf(P.sb([128, 4096], BF16))
            mbp = Ring([Buf(P.sb([128, 1024], BF16)) for _ in range(2)])
            rbr = Ring([Buf(P.sb([128, 512], F32)) for _ in range(2)])
            qir = Ring([Buf(P.sb([128, 8, 128], BF16), P.chan()) for _ in range(2)])
            sm = {n: Buf(P.sb([128, 1], F32)) for n in ("lo", "hi", "tau", "step", "cand", "cnt", "gs")}
            cntp = Buf(P.sb([128, 4], F32))
        else:
            mb = cx.load_const(mbd, [128, NMASK, 128], BF16)

        def attn_group(qb, heads0_list, items, g, ot, first_g, hooks=None):
            n = len(items)
            for it in range(n):
                if hooks and it in hooks:
                    hooks[it]()
                (qsel, kb_, kidx, vb_, vidx, map_, mdep) = items[it]()
                ps = psS.next()
                q4 = qb.t[:, qsel:qsel + 4, :].rearrange("p h q -> p (h q)")
                P.op("pe", (lambda ps=ps, kb_=kb_, kidx=kidx, q4=q4: lambda e: e.matmul(
                    ps.t[:], lhsT=kb_.t[:, kidx, :], rhs=q4, start=True, stop=False))(),
                    reads=[kb_.d, qb.d], writes=[ps.d])
                for hh in range(4):
                    P.op("pe", (lambda ps=ps, hh=hh, map_=map_: lambda e: e.matmul(
                        ps.t[:, hh * 128:(hh + 1) * 128], lhsT=ident.t[:], rhs=map_, start=False, stop=(hh == 3)))(),
                        reads=[ident.d, mdep], pwrites=[ps.d])
                pt = pTr.next()
                P.op("act", (lambda ps=ps, pt=pt: lambda e: e.activation(out=pt.t[:], in_=ps.t[:], func=AF.Exp, scale=SCALE))(),
                     reads=[ps.d], writes=[pt.d])
                for hh in range(4):
                    P.op("pe", (lambda pt=pt, hh=hh, vb_=vb_, vidx=vidx, it=it: lambda e: e.matmul(
                        acc[hh].t[:, 0:129], lhsT=pt.t[:, hh * 128:(hh + 1) * 128], rhs=vb_.t[:, vidx, 0:129],
                        start=(it == 0), stop=(it == n - 1)))(),
                        reads=[pt.d, vb_.d], **{W(it == 0): [acc[hh].d]})
            for hh in range(4):
                sg = stg.next()
                rc = rcp.next()
                P.op("act", (lambda sg=sg, hh=hh: lambda e: e.copy(out=sg.t[:, 0:129], in_=acc[hh].t[:, 0:129]))(),
                     reads=[acc[hh].d], writes=[sg.d])
                P.op("dve", (lambda sg=sg, rc=rc: lambda e: e.reciprocal(out=rc.t[:], in_=sg.t[:, 128:129]))(),
                     reads=[sg.d], writes=[rc.d])
                c0 = (4 * g + hh) * 128
                P.op("dve", (lambda sg=sg, rc=rc, c0=c0: lambda e: e.tensor_scalar(
                    out=ot.t[:, c0:c0 + 128], in0=sg.t[:, 0:128], scalar1=rc.t[:], scalar2=None, op0=ALU.mult))(),
                    reads=[sg.d, rc.d], **{W(first_g and hh == 0): [ot.d]})

        for j in range(NB):
            qb = qring.next()
            P.dma("sp", (lambda qb=qb, j=j: lambda e: e.dma_start(out=qb.t[:], in_=qT[j]))(), qb.c, writes=[qb.d])
            if dsa:
                L = 1024 * (j + 1)
                qib = qir.next()
                P.dma("sp", (lambda qib=qib, j=j: lambda e: e.dma_start(out=qib.t[:], in_=qiT[j]))(), qib.c, writes=[qib.d])
                for c in range(L // 512):
                    for h in range(16):
                        hp, eo = h // 2, h % 2
                        P.op("pe", (lambda qib=qib, hp=hp, eo=eo, c=c: lambda e: e.matmul(
                            psI.t[:], lhsT=qib.t[64 * eo:64 * eo + 64, hp, :], rhs=ki.t[64 * eo:64 * eo + 64, c * 512:(c + 1) * 512],
                            start=True, stop=True))(), reads=[qib.d, ki.d], writes=[psI.d])
                        rb = rbr.next()
                        P.op("act", (lambda rb=rb: lambda e: e.activation(out=rb.t[:], in_=psI.t[:], func=AF.Relu))(),
                             reads=[psI.d], writes=[rb.d])
                        if h == 0:
                            P.op("dve", (lambda rb=rb, c=c, j=j: lambda e: e.tensor_scalar(
                                out=isc.t[:, c * 512:(c + 1) * 512], in0=rb.t[:], scalar1=wif.t[:, j, 0:1], scalar2=None, op0=ALU.mult))(),
                                reads=[rb.d, wif.d], **{W(c == 0): [isc.d]})
                        else:
                            P.op("dve", (lambda rb=rb, c=c, j=j, h=h: lambda e: e.scalar_tensor_tensor(
                                out=isc.t[:, c * 512:(c + 1) * 512], in0=rb.t[:], scalar=wif.t[:, j, h:h + 1],
                                in1=isc.t[:, c * 512:(c + 1) * 512], op0=ALU.mult, op1=ALU.add))(),
                                reads=[rb.d, wif.d], pwrites=[isc.d])
                P.op("dve", (lambda L=L: lambda e: e.tensor_reduce(out=sm["lo"].t[:], in_=isc.t[:, 0:L], axis=AX.X, op=ALU.min))(),
                     reads=[isc.d], writes=[sm["lo"].d])
                P.op("dve", (lambda L=L: lambda e: e.tensor_tensor(out=isc.t[:, L - 1024:L], in0=isc.t[:, L - 1024:L], in1=cb.t[:], op=ALU.add))(),
                     reads=[cb.d, sm["lo"].d], pwrites=[isc.d])
                P.op("dve", (lambda L=L: lambda e: e.tensor_reduce(out=sm["hi"].t[:], in_=isc.t[:, 0:L], axis=AX.X, op=ALU.max))(),
                     reads=[isc.d], writes=[sm["hi"].d])
                P.op("dve", lambda e: e.tensor_scalar(out=sm["tau"].t[:], in0=sm["lo"].t[:], scalar1=-1.0, scalar2=None, op0=ALU.add),
                     reads=[sm["lo"].d], writes=[sm["tau"].d])
                P.op("dve", lambda e: e.tensor_tensor(out=sm["step"].t[:], in0=sm["hi"].t[:], in1=sm["tau"].t[:], op=ALU.subtract),
                     reads=[sm["hi"].d, sm["tau"].d], writes=[sm["step"].d])
                P.op("dve", lambda e: e.tensor_scalar(out=sm["step"].t[:], in0=sm["step"].t[:], scalar1=0.5, scalar2=None, op0=ALU.mult),
                     reads=[sm["step"].d], writes=[sm["step"].d])
                npc = (L + 4095) // 4096
                for itn in range(BISECT_ITERS):
                    P.op("dve", lambda e: e.tensor_tensor(out=sm["cand"].t[:], in0=sm["tau"].t[:], in1=sm["step"].t[:], op=ALU.add),
                         reads=[sm["tau"].d, sm["step"].d], writes=[sm["cand"].d])
                    for pi in range(npc):
                        a0, a1 = pi * 4096, min(L, (pi + 1) * 4096)
                        P.op("dve", (lambda a0=a0, a1=a1, pi=pi: lambda e: e.tensor_scalar(
                            out=junk.t[:, 0:a1 - a0], in0=isc.t[:, a0:a1], scalar1=sm["cand"].t[:], scalar2=0.0,
                            op0=ALU.is_gt, op1=ALU.add, accum_out=cntp.t[:, pi:pi + 1]))(),
                            reads=[isc.d, sm["cand"].d], **({"writes": [junk.d, cntp.d]} if pi == 0 else {"writes": [junk.d], "pwrites": [cntp.d]}))
                    P.op("dve", (lambda npc=npc: lambda e: e.tensor_reduce(out=sm["cnt"].t[:], in_=cntp.t[:, 0:npc], axis=AX.X, op=ALU.add))(),
                         reads=[cntp.d], writes=[sm["cnt"].d])
                    P.op("dve", lambda e: e.tensor_scalar(out=sm["gs"].t[:], in0=sm["cnt"].t[:], scalar1=float(TOPK) - 0.5, scalar2=sm["step"].t[:],
                                                         op0=ALU.is_ge, op1=ALU.mult),
                         reads=[sm["cnt"].d, sm["step"].d], writes=[sm["gs"].d])
                    P.op("dve", lambda e: e.tensor_tensor(out=sm["tau"].t[:], in0=sm["tau"].t[:], in1=sm["gs"].t[:], op=ALU.add),
                         reads=[sm["gs"].d], writes=[sm["tau"].d])
                    P.op("dve", lambda e: e.tensor_scalar(out=sm["step"].t[:], in0=sm["step"].t[:], scalar1=0.5, scalar2=None, op0=ALU.mult),
                         reads=[sm["gs"].d], writes=[sm["step"].d])
                for pc in range(L // 1024):
                    m = mbp.next()
                    P.op("dve", (lambda m=m, pc=pc: lambda e: e.tensor_scalar(
                        out=m.t[:], in0=isc.t[:, pc * 1024:(pc + 1) * 1024], scalar1=sm["tau"].t[:], scalar2=NEG,
                        op0=ALU.is_le, op1=ALU.mult))(), reads=[isc.d, sm["tau"].d], writes=[m.d])
                    for i in range(8):
                        P.op("pe", (lambda m=m, i=i: lambda e: e.transpose(out=psT.t[:, i, :], in_=m.t[:, i * 128:(i + 1) * 128], identity=ident.t[:]))(),
                             reads=[m.d, ident.d], **{W(i == 0): [psT.d]})
                    P.op("act", (lambda pc=pc: lambda e: e.copy(out=mbT.t[:, pc * 8:(pc + 1) * 8, :], in_=psT.t[:]))(),
                         reads=[psT.d], **{W(pc == 0): [mbT.d]})
            ot = otok.next()
            for g in range(4):
                hooks = None
                if dsa:
                    nkb = 8 * (j + 1)
                    nsub = (nkb + 15) // 16
                    bufs = {}

                    def load_sub(n, g=g, bufs=bufs):
                        kb_ = kring.next()
                        vb_ = vring.next()
                        s0 = n * 16
                        P.dma("sp", lambda e: e.dma_start(out=kb_.t[:], in_=kT[g, :, s0:s0 + 16, :]), kb_.c, writes=[kb_.d])
                        P.dma("sp", lambda e: e.dma_start(out=vb_.t[:], in_=va[:, s0:s0 + 16, g, :]), vb_.c, writes=[vb_.d])
                        bufs[n] = (kb_, vb_)
                    load_sub(0)
                    hooks = {}
                    for n in range(nsub - 1):
                        hooks[n * 16] = (lambda n=n: load_sub(n + 1))
                    flat = [(lambda kbi=kbi, g=g, bufs=bufs: (4 * g, bufs[kbi // 16][0], kbi % 16, bufs[kbi // 16][1], kbi % 16,
                                                              mbT.t[:, kbi, :], mbT.d)) for kbi in range(nkb)]
                else:
                    kb_ = kring.next()
                    vb_ = vring.next()
                    P.dma("sp", (lambda kb_=kb_, g=g, j=j: lambda e: e.dma_start(out=kb_.t[:], in_=kT[g, :, j, :, :]))(), kb_.c, writes=[kb_.d])
                    P.dma("sp", (lambda vb_=vb_, g=g, j=j: lambda e: e.dma_start(out=vb_.t[:], in_=va[:, j, :, g, :]))(), vb_.c, writes=[vb_.d])
                    flat = []
                    mi = 0
                    for dg, (win, dil, rels) in enumerate(DIL_GROUPS):
                        for rel in rels:
                            flat.append((lambda dg=dg, g=g, kb_=kb_, vb_=vb_, rel=rel, mi=mi: (dg * 16 + 4 * g, kb_, rel, vb_, rel, mb.t[:, mi, :], mb.d)))
                            mi += 1
                attn_group(qb, None, flat, g, ot, g == 0, hooks)
            P.dma("sp", (lambda ot=ot, j=j: lambda e: e.dma_start(out=od[j * 128:(j + 1) * 128, :], in_=ot.t[:]))(), ot.c, reads=[ot.d])
        P.emit(final_waits=[b.c for b in otok.bufs])
    return nc


def dil_mask_np():
    ps = np.arange(128)[:, None]
    pq = np.arange(128)[None, :]
    tiles = []
    for (win, dil, rels) in DIL_GROUPS:
        for rel in rels:
            dlt = 128 * (16 - rel) + pq - ps
            ok = (dlt >= 0) & (dlt <= win) & (dlt % dil == 0)
            tiles.append(np.where(ok, 0.0, NEG).astype(np.float32))
    return np.ascontiguousarray(np.stack(tiles, axis=1)).astype(NPBF)


def causal_bias_np(c):
    pq = np.arange(128)[:, None]
    r = np.arange(8)[None, :, None]
    ps = np.arange(128)[None, None, :]
    ok = (r < c) | ((r == c) & (ps <= pq[:, :, None]))
    return np.where(ok, 0.0, -1e30).astype(np.float32).reshape(128, 1024)


def prep_dsa_inputs(proj_list):
    kg = from_local([p[:, 2048:2560] for p in proj_list])
    vg = from_local([p[:, 2560:3072] for p in proj_list])
    kig = from_local([p[:, 4096:4160] for p in proj_list])
    kT = np.ascontiguousarray(kg.reshape(128, 128, 4, 128).transpose(2, 3, 0, 1))
    va = np.zeros((128, 128, 4, 132), dtype=NPBF)
    va[:, :, :, 0:128] = vg.reshape(128, 128, 4, 128).transpose(1, 0, 2, 3)
    va[:, :, :, 128] = 1.0
    kiT2 = np.ascontiguousarray(np.concatenate([kig.T, kig.T], axis=0))
    maps = []
    for c in range(NCORES):
        p = proj_list[c]
        qT = np.ascontiguousarray(p[:, 0:2048].reshape(NB, 128, 16, 128).transpose(0, 3, 2, 1))
        qiT = np.ascontiguousarray(p[:, 3072:4096].reshape(NB, 128, 8, 2, 64).transpose(0, 3, 4, 2, 1).reshape(NB, 128, 8, 128))
        wi = np.ascontiguousarray(p[:, 4160:4176])
        maps.append({"qT": qT, "ident": _IDENT, "kT": kT, "va": va, "qiT": qiT, "kiT2": kiT2, "wi": wi,
                     "cbias": causal_bias_np(c)})
    return maps


def prep_dil_inputs(q_list, kv_list):
    kg = from_local([p[:, 0:512] for p in kv_list])
    vg = from_local([p[:, 512:1024] for p in kv_list])
    kpad = np.concatenate([np.zeros((16 * 128, 512), dtype=NPBF), kg], axis=0).reshape(144, 128, 4, 128)
    vpad = np.zeros((144, 128, 4, 132), dtype=NPBF)
    vpad[16:, :, :, 0:128] = vg.reshape(128, 128, 4, 128)
    vpad[16:, :, :, 128] = 1.0
    mb = dil_mask_np()
    maps = []
    for c in range(NCORES):
        idx = (8 * np.arange(NB)[:, None] + c) + np.arange(17)[None, :]
        kT = np.ascontiguousarray(kpad[idx].transpose(3, 4, 0, 1, 2))
        va = np.ascontiguousarray(vpad[idx].transpose(2, 0, 1, 3, 4))
        qT = np.ascontiguousarray(q_list[c].reshape(NB, 128, 48, 128).transpose(0, 3, 2, 1))
        maps.append({"qT": qT, "ident": _IDENT, "kT": kT, "va": va, "mbias": mb})
    return maps


def to_oT(o_loc):
    return np.ascontiguousarray(o_loc.reshape(NTL, 16, 128).transpose(2, 1, 0))


def rope_tables_np(pos, rot_dim):
    inv = (500000.0 ** (-(np.arange(0, rot_dim, 2, dtype=np.float32)) / np.float32(rot_dim))).astype(np.float32)
    ang = pos.astype(np.float32)[:, None] * inv[None, :]
    return np.cos(ang).astype(np.float32), np.sin(ang).astype(np.float32)


def local_positions(c):
    j = np.arange(NB)[:, None]
    p = np.arange(128)[None, :]
    return ((8 * j + c) * 128 + p).reshape(-1)


def to_local(a, c):
    return np.ascontiguousarray(a.reshape(NB, NCORES, 128, *a.shape[1:])[:, c].reshape(NTL, *a.shape[1:]))


def from_local(parts):
    a = np.stack([p.reshape(NB, 128, *p.shape[1:]) for p in parts], axis=1)
    return np.ascontiguousarray(a.reshape(S, *parts[0].shape[1:]))


_IDENT = np.eye(128, dtype=np.float32).astype(NPBF)


def run(nc, in_maps):
    return run_bass_kernel_spmd(nc, in_maps, core_ids=list(range(NCORES))).results


def launch_proj(x_loc_list, w, g, w_cols, segs, name):
    nc = build_proj(w_cols, segs, name)
    in_maps = []
    for c in range(NCORES):
        pos = local_positions(c)
        ch, sh = rope_tables_np(pos, 32)
        ci, si = rope_tables_np(pos, 16)
        in_maps.append({"x": x_loc_list[c], "w": w, "g": g.reshape(1, D), "ident": _IDENT,
                        "cos_h": ch, "sin_h": sh, "cos_i": ci, "sin_i": si})
    res = run(nc, in_maps)
    return [r["proj"] for r in res]


SEGS_A = [(0, 2048, "rope_h"), (2048, 512, "rope_h"), (2560, 512, "plain"), (3072, 1024, "rope_i"), (4096, 80, "ki_wi")]


SEGS_KV = [(0, 512, "rope_h"), (512, 512, "plain")]
SEGS_Q2 = [(0, 6144, "rope_h")]


def kernel(**inputs):
    x = np.ascontiguousarray(np.asarray(inputs["x"], dtype=np.float32)[0])
    f = lambda k: np.ascontiguousarray(np.asarray(inputs[k], dtype=np.float32))
    x_list = [to_local(x, c) for c in range(NCORES)]
    proj = launch_proj(x_list, f("a_w_in")[0], f("a_attn_norm")[0], A_IN_W, SEGS_A, "a")
    res = run(build_attn("dsa"), prep_dsa_inputs(proj))
    o1 = [to_oT(r["o"]) for r in res]
    h1, _, _ = launch_mlp(o1, x_list, f("a_w_out")[0], f("mlp_norm")[0], f("w_up")[0], f("w_down")[0], f("final_norm"), False)
    kv = launch_proj(h1, f("w_kv"), f("kv_norm"), 1024, SEGS_KV, "kv")
    q2 = launch_proj(h1, f("b_w_q")[0], f("b_attn_norm")[0], 6144, SEGS_Q2, "q2")
    res = run(build_attn("dil"), prep_dil_inputs(q2, kv))
    o2 = [to_oT(r["o"]) for r in res]
    _, y, _ = launch_mlp(o2, h1, f("b_w_out")[0], f("mlp_norm")[1], f("w_up")[1], f("w_down")[1], f("final_norm"), True)
    return from_local(y)[None].astype(np.float32)
```
